# Optimizing a Trainium2 kernel written in Bass

```python
import jax
import jax.numpy as jnp
from jax import lax
import numpy as np


D_MODEL = 4096
BATCH = 2
SEQ = 4096
DEPTH = 4

CHUNK = 64
N_GROUPS = 4
GROUP_WIDTH = D_MODEL // N_GROUPS

HGRN_HEADS = 8
HGRN_KEY = GROUP_WIDTH // HGRN_HEADS
HGRN_VAL = GROUP_WIDTH // HGRN_HEADS

LRU_WIDTH = GROUP_WIDTH
LRU_BLOCKS = 8
LRU_BLOCK = LRU_WIDTH // LRU_BLOCKS
CONV_WIDTH = 4
LRU_C = 8.0

GLA_HEADS = 4
GLA_KEY = GROUP_WIDTH // 2 // GLA_HEADS
GLA_VAL = GROUP_WIDTH // GLA_HEADS
GLA_RANK = 16
GLA_NORMALIZER = 16.0

RET_HEADS = 8
RET_KEY = GROUP_WIDTH // RET_HEADS
RET_VAL = GROUP_WIDTH // RET_HEADS
ROPE_BASE = 10000.0

D_FF = 2 * D_MODEL
N_EXPERTS = 8
TOP_K = 2
D_FF_EXPERT = D_MODEL // 2
N_DENSE = (DEPTH + 1) // 2
N_MOE = DEPTH // 2

DEEPNORM_ALPHA = (2 * DEPTH) ** 0.25
DEEPNORM_BETA = (8 * DEPTH) ** -0.25
LN_EPS = 1e-5
RMS_EPS = 1e-6

IN_SPLITS = (
    HGRN_HEADS * HGRN_KEY, HGRN_HEADS * HGRN_KEY, HGRN_HEADS * HGRN_VAL, HGRN_HEADS * HGRN_VAL,
    LRU_WIDTH, LRU_WIDTH,
    GLA_HEADS * GLA_KEY, GLA_HEADS * GLA_KEY, GLA_HEADS * GLA_VAL, GLA_HEADS * GLA_VAL, GLA_RANK,
    RET_HEADS * RET_KEY, RET_HEADS * RET_KEY, RET_HEADS * RET_VAL, RET_HEADS * RET_VAL,
)
D_IN = sum(IN_SPLITS)

kernel_name = 'hybrid_parallel_recurrent_mixers_deepnorm_moe'


def layer_norm(x, g, b):
    xf = x.astype(jnp.float32)
    mu = jnp.mean(xf, axis=-1, keepdims=True)
    var = jnp.mean(jnp.square(xf - mu), axis=-1, keepdims=True)
    return ((xf - mu) * lax.rsqrt(var + LN_EPS) * g + b).astype(x.dtype)


def rms_norm(x, g):
    return x * lax.rsqrt(jnp.mean(jnp.square(x), axis=-1, keepdims=True) + RMS_EPS) * g


def group_layer_norm(x, g):
    mu = jnp.mean(x, axis=-1, keepdims=True)
    var = jnp.mean(jnp.square(x - mu), axis=-1, keepdims=True)
    return (x - mu) * lax.rsqrt(var + LN_EPS) * g


def split_heads(a, n_heads):
    b, t, _ = a.shape
    return a.reshape(b, t, n_heads, -1).transpose(0, 2, 1, 3)


def merge_heads(a):
    b, h, t, d = a.shape
    return a.transpose(0, 2, 1, 3).reshape(b, t, h * d)


def to_chunks(a):
    b, h, t, d = a.shape
    return a.reshape(b, h, t // CHUNK, CHUNK, d)


def gla_chunk_scan(q, k, v, log_g):
    b, h, t, kd = q.shape
    vd = v.shape[-1]
    xs = tuple(jnp.moveaxis(to_chunks(a.astype(jnp.float32)), 2, 0) for a in (q, k, v, log_g))
    causal = jnp.tril(jnp.ones((CHUNK, CHUNK), dtype=bool))

    def step(state, inp):
        q_c, k_c, v_c, g_c = inp
        cum = jnp.cumsum(g_c, axis=-2)
        diff = cum[..., :, None, :] - cum[..., None, :, :]
        decay = jnp.exp(jnp.where(causal[:, :, None], diff, -jnp.inf))
        scores = jnp.einsum('bhtk,bhsk,bhtsk->bhts', q_c, k_c, decay)
        out = jnp.einsum('bhts,bhsv->bhtv', scores, v_c) + jnp.einsum('bhtk,bhkv->bhtv', q_c * jnp.exp(cum), state)
        last = cum[..., -1:, :]
        state = jnp.exp(last[..., 0, :])[..., None] * state + jnp.einsum('bhsk,bhsv->bhkv', k_c * jnp.exp(last - cum), v_c)
        return state, out

    _, o = lax.scan(step, jnp.zeros((b, h, kd, vd), jnp.float32), xs)
    return jnp.moveaxis(o, 0, 2).reshape(b, h, t, vd)


def rotary(x):
    t, d = x.shape[-2], x.shape[-1]
    half = d // 2
    inv_freq = ROPE_BASE ** (-jnp.arange(half, dtype=jnp.float32) / half)
    ang = jnp.arange(t, dtype=jnp.float32)[:, None] * inv_freq[None, :]
    cos, sin = jnp.cos(ang), jnp.sin(ang)
    x1, x2 = x[..., :half], x[..., half:]
    return jnp.concatenate([x1 * cos - x2 * sin, x1 * sin + x2 * cos], axis=-1)


def retention_chunkwise(q, k, v, log_gamma):
    b, h, t, kd = q.shape
    vd = v.shape[-1]
    qc, kc, vc = to_chunks(q), to_chunks(k), to_chunks(v)
    pos = jnp.arange(CHUNK, dtype=jnp.float32)
    rel = pos[:, None] - pos[None, :]
    intra = jnp.where(rel >= 0, jnp.exp(log_gamma[:, None, None] * jnp.maximum(rel, 0.0)), 0.0)
    scores = jnp.einsum('bhntk,bhnsk->bhnts', qc, kc) * intra[None, :, None]
    o_intra = jnp.einsum('bhnts,bhnsv->bhntv', scores, vc)
    k_decay = jnp.exp(log_gamma[:, None] * (CHUNK - 1.0 - pos)[None, :])
    chunk_kv = jnp.einsum('bhnsk,hs,bhnsv->nbhkv', kc, k_decay, vc)
    chunk_decay = jnp.exp(log_gamma * CHUNK)[None, :, None, None]

    def step(state, kv):
        return chunk_decay * state + kv, state

    _, states_before = lax.scan(step, jnp.zeros((b, h, kd, vd), jnp.float32), chunk_kv)
    q_decay = jnp.exp(log_gamma[:, None] * (pos + 1.0)[None, :])
    o_inter = jnp.einsum('bhntk,ht,nbhkv->bhntv', qc, q_decay, states_before)
    return (o_intra + o_inter).reshape(b, h, t, vd)


def causal_depthwise_conv(x, w, bias):
    y = lax.conv_general_dilated(x, w.astype(jnp.float32)[:, None, :], window_strides=(1,),
                                 padding=[(CONV_WIDTH - 1, 0)], dimension_numbers=('NWC', 'WIO', 'NWC'),
                                 feature_group_count=x.shape[-1])
    return y + bias


def rg_lru(x, w_a, b_a, w_x, b_x, lam):
    b, t, w = x.shape
    xb = x.reshape(b, t, LRU_BLOCKS, LRU_BLOCK)
    r = jax.nn.sigmoid(jnp.einsum('btni,nij->btnj', xb, w_a).reshape(b, t, w) + b_a)
    i = jax.nn.sigmoid(jnp.einsum('btni,nij->btnj', xb, w_x).reshape(b, t, w) + b_x)
    log_a = -LRU_C * r * jax.nn.softplus(-lam)
    a = jnp.exp(log_a)
    u = jnp.sqrt(-jnp.expm1(2.0 * log_a)) * (i * x)

    def combine(left, right):
        a_l, u_l = left
        a_r, u_r = right
        return a_l * a_r, a_r * u_l + u_r

    _, hs = lax.associative_scan(combine, (a, u), axis=1)
    return hs


def hybrid_mixer(h, lower_bound, w_in, w_out, hgrn_norm, conv_w, conv_b, lru_wa, lru_ba, lru_wx, lru_bx,
                 lru_lambda, gla_w_gate, gla_b_gate, gla_norm, ret_norm):
    proj = jnp.einsum('btd,de->bte', h, w_in).astype(jnp.float32)
    idx = [int(i) for i in np.cumsum(IN_SPLITS)[:-1]]
    (a_q, a_f, a_i, a_g, b_x, b_y, c_q, c_k, c_v, c_g, c_r,
     d_q, d_k, d_v, d_g) = jnp.split(proj, idx, axis=-1)

    f = lower_bound + (1.0 - lower_bound) * jax.nn.sigmoid(a_f)
    o_a = gla_chunk_scan(split_heads(jax.nn.silu(a_q), HGRN_HEADS) * HGRN_KEY ** -0.5,
                         split_heads(1.0 - f, HGRN_HEADS), split_heads(a_i, HGRN_HEADS),
                         split_heads(jnp.log(f), HGRN_HEADS))
    o_a = merge_heads(rms_norm(o_a, hgrn_norm)) * jax.nn.silu(a_g)

    x_conv = causal_depthwise_conv(b_x, conv_w, conv_b)
    o_b = rg_lru(x_conv, lru_wa, lru_ba, lru_wx, lru_bx, lru_lambda) * jax.nn.gelu(b_y)

    log_alpha = jax.nn.log_sigmoid(jnp.matmul(c_r, gla_w_gate) + gla_b_gate) / GLA_NORMALIZER
    o_c = gla_chunk_scan(split_heads(c_q, GLA_HEADS) * GLA_KEY ** -0.5, split_heads(c_k, GLA_HEADS),
                         split_heads(c_v, GLA_HEADS), split_heads(log_alpha, GLA_HEADS))
    o_c = merge_heads(rms_norm(o_c, gla_norm)) * jax.nn.silu(c_g)

    log_gamma = jnp.log1p(-(2.0 ** (-5.0 - jnp.arange(RET_HEADS, dtype=jnp.float32))))
    q_r = rotary(split_heads(d_q, RET_HEADS))
    k_r = rotary(split_heads(d_k, RET_HEADS)) * RET_KEY ** -0.5
    o_d = retention_chunkwise(q_r, k_r, split_heads(d_v, RET_HEADS), log_gamma)
    o_d = merge_heads(group_layer_norm(o_d, ret_norm)) * jax.nn.silu(d_g)

    mixed = jnp.concatenate([o_a, o_b, o_c, o_d], axis=-1)
    return jnp.einsum('bte,ed->btd', mixed, w_out).astype(h.dtype)


def swiglu(h, w_gate, w_up, w_down):
    return jnp.matmul(jax.nn.silu(jnp.matmul(h, w_gate)) * jnp.matmul(h, w_up), w_down)


def moe_swiglu(h, router_w, w_gate, w_up, w_down):
    b, t, d = h.shape
    tok = h.reshape(b * t, d)
    logits = jnp.matmul(tok, router_w).astype(jnp.float32)
    top_logits, top_idx = lax.top_k(logits, TOP_K)
    top_w = jax.nn.softmax(top_logits, axis=-1)
    gates = jnp.einsum('tk,tke->te', top_w, jax.nn.one_hot(top_idx, N_EXPERTS, dtype=jnp.float32))
    hid = jax.nn.silu(jnp.einsum('td,edf->tef', tok, w_gate)) * jnp.einsum('td,edf->tef', tok, w_up)
    y = jnp.einsum('tef,te,efd->td', hid, gates.astype(hid.dtype), w_down)
    return y.reshape(b, t, d).astype(h.dtype)


def setup_inputs(seed: int = 0) -> dict:
    key = jax.random.key(seed)
    ks = jax.random.split(key, 32)

    def nrm(k, shape, scale):
        return jax.random.normal(k, shape, jnp.float32) * scale

    u = jax.random.uniform(ks[10], (DEPTH, LRU_WIDTH), jnp.float32, minval=0.9, maxval=0.999)
    p = u ** (1.0 / LRU_C)
    return {
        'x': nrm(ks[0], (BATCH, SEQ, D_MODEL), 1.0),
        'w_in': nrm(ks[1], (DEPTH, D_MODEL, D_IN), D_MODEL ** -0.5),
        'w_out': nrm(ks[2], (DEPTH, D_MODEL, D_MODEL), DEEPNORM_BETA * D_MODEL ** -0.5),
        'hgrn_lb': 1.0 + nrm(ks[3], (DEPTH, HGRN_HEADS * HGRN_KEY), 0.1),
        'hgrn_norm': 1.0 + nrm(ks[4], (HGRN_VAL,), 0.02) + jnp.zeros((DEPTH, 1), jnp.float32) + nrm(ks[26], (DEPTH, HGRN_VAL), 0.02),
        'conv_w': nrm(ks[5], (DEPTH, CONV_WIDTH, LRU_WIDTH), CONV_WIDTH ** -0.5),
        'conv_b': nrm(ks[6], (DEPTH, LRU_WIDTH), 0.02),
        'lru_wa': nrm(ks[7], (DEPTH, LRU_BLOCKS, LRU_BLOCK, LRU_BLOCK), LRU_BLOCK ** -0.5),
        'lru_ba': nrm(ks[8], (DEPTH, LRU_WIDTH), 0.02),
        'lru_wx': nrm(ks[9], (DEPTH, LRU_BLOCKS, LRU_BLOCK, LRU_BLOCK), LRU_BLOCK ** -0.5),
        'lru_bx': nrm(ks[11], (DEPTH, LRU_WIDTH), 0.02),
        'lru_lambda': jnp.log(p) - jnp.log1p(-p),
        'gla_w_gate': nrm(ks[12], (DEPTH, GLA_RANK, GLA_HEADS * GLA_KEY), GLA_RANK ** -0.5),
        'gla_b_gate': nrm(ks[13], (DEPTH, GLA_HEADS * GLA_KEY), 0.02),
        'gla_norm': 1.0 + nrm(ks[14], (DEPTH, GLA_VAL), 0.02),
        'ret_norm': 1.0 + nrm(ks[15], (DEPTH, RET_VAL), 0.02),
        'ln1_g': 1.0 + nrm(ks[16], (DEPTH, D_MODEL), 0.02),
        'ln1_b': nrm(ks[17], (DEPTH, D_MODEL), 0.02),
        'ln2_g': 1.0 + nrm(ks[18], (DEPTH, D_MODEL), 0.02),
        'ln2_b': nrm(ks[19], (DEPTH, D_MODEL), 0.02),
        'ffn_w_gate': nrm(ks[20], (N_DENSE, D_MODEL, D_FF), D_MODEL ** -0.5),
        'ffn_w_up': nrm(ks[21], (N_DENSE, D_MODEL, D_FF), D_MODEL ** -0.5),
        'ffn_w_down': nrm(ks[22], (N_DENSE, D_FF, D_MODEL), DEEPNORM_BETA * D_FF ** -0.5),
        'router_w': nrm(ks[23], (N_MOE, D_MODEL, N_EXPERTS), D_MODEL ** -0.5),
        'exp_w_gate': nrm(ks[24], (N_MOE, N_EXPERTS, D_MODEL, D_FF_EXPERT), D_MODEL ** -0.5),
        'exp_w_up': nrm(ks[25], (N_MOE, N_EXPERTS, D_MODEL, D_FF_EXPERT), D_MODEL ** -0.5),
        'exp_w_down': nrm(ks[27], (N_MOE, N_EXPERTS, D_FF_EXPERT, D_MODEL), DEEPNORM_BETA * D_FF_EXPERT ** -0.5),
    }


def reference(x, w_in, w_out, hgrn_lb, hgrn_norm, conv_w, conv_b, lru_wa, lru_ba, lru_wx, lru_bx, lru_lambda,
              gla_w_gate, gla_b_gate, gla_norm, ret_norm, ln1_g, ln1_b, ln2_g, ln2_b, ffn_w_gate, ffn_w_up,
              ffn_w_down, router_w, exp_w_gate, exp_w_up, exp_w_down):
    lb_soft = jax.nn.softmax(hgrn_lb.astype(jnp.float32), axis=0)
    lower_bounds = jnp.cumsum(lb_soft, axis=0) - lb_soft[0]
    h = x
    for layer in range(DEPTH):
        mix = hybrid_mixer(h, lower_bounds[layer], w_in[layer], w_out[layer], hgrn_norm[layer], conv_w[layer],
                           conv_b[layer], lru_wa[layer], lru_ba[layer], lru_wx[layer], lru_bx[layer],
                           lru_lambda[layer], gla_w_gate[layer], gla_b_gate[layer], gla_norm[layer], ret_norm[layer])
        h = layer_norm(DEEPNORM_ALPHA * h + mix, ln1_g[layer], ln1_b[layer])
        j = layer // 2
        if layer % 2 == 0:
            ff = swiglu(h, ffn_w_gate[j], ffn_w_up[j], ffn_w_down[j])
        else:
            ff = moe_swiglu(h, router_w[j], exp_w_gate[j], exp_w_up[j], exp_w_down[j])
        h = layer_norm(DEEPNORM_ALPHA * h + ff, ln2_g[layer], ln2_b[layer])
    return h
```

```python
import contextlib
import numpy as np
import concourse.bass as bass
import concourse.mybir as mybir
from concourse.bass_utils import run_bass_kernel_spmd

F32 = mybir.dt.float32
BF16 = mybir.dt.bfloat16
AF = mybir.ActivationFunctionType
ALU = mybir.AluOpType

D_MODEL = 4096
BATCH = 2
SEQ = 4096
DEPTH = 4
NCORES = 8
TT = 512
KC = D_MODEL // 128
ALPHA = float((2 * DEPTH) ** 0.25)
LN_EPS = 1e-5
RMS_EPS = 1e-6
D_FF = 2 * D_MODEL
N_EXP = 8
D_FFE = D_MODEL // 2
DEBUG = False


class Buf:
    __slots__ = ("ap", "name", "writer", "readers", "dsem", "dcount")

    def __init__(self, ap, name=""):
        self.ap = ap
        self.name = name
        self.writer = None
        self.readers = []
        self.dsem = None
        self.dcount = 0


class Sched:
    COMPUTE = ("pe", "act", "dve", "pool")

    def __init__(self, nc, stack):
        self.nc = nc
        self.stack = stack
        self.eng = {"pe": nc.tensor, "act": nc.scalar, "dve": nc.vector,
                    "pool": nc.gpsimd, "sp": nc.sync}
        self.sems = {}
        self.count = {}
        for e in self.COMPUTE:
            self.sems[e] = stack.enter_context(nc.semaphore("s_" + e))
            self.count[e] = 0
        self.waited = {}
        self.nsem = 0
        self.out_tokens = []

    def _wait(self, engine, tok):
        if tok is None:
            return
        key, val = tok
        if self.waited.get((engine, key), 0) >= val:
            return
        self.waited[(engine, key)] = val
        self.eng[engine].wait_ge(self.sems[key], val)

    def _deps(self, engine, reads, writes):
        toks = []
        for b in reads:
            if b.writer is not None:
                toks.append(b.writer)
        for b in writes:
            if b.writer is not None:
                toks.append(b.writer)
            toks.extend(b.readers)
        best = {}
        for key, val in toks:
            if engine == "pe" and key == "pe":
                continue
            if best.get(key, 0) < val:
                best[key] = val
        for key, val in best.items():
            self._wait(engine, (key, val))

    def _commit(self, tok, reads, writes):
        for b in reads:
            b.readers.append(tok)
            if len(b.readers) > 24:
                best = {}
                for k, v in b.readers:
                    if best.get(k, 0) < v:
                        best[k] = v
                b.readers = list(best.items())
        for b in writes:
            b.writer = tok
            b.readers = []

    def op(self, engine, reads, writes, fn):
        self._deps(engine, reads, writes)
        ins = fn(self.eng[engine])
        self.count[engine] += 1
        ins.then_inc(self.sems[engine], 1)
        tok = (engine, self.count[engine])
        self._commit(tok, reads, writes)
        return tok

    def dma(self, queue, out_buf, in_buf, out_ap=None, in_ap=None, sem_buf=None, n=1, fn=None):
        sb = sem_buf if sem_buf is not None else out_buf
        if sb.dsem is None:
            key = "d%d" % self.nsem
            self.nsem += 1
            self.sems[key] = self.stack.enter_context(self.nc.semaphore(key))
            sb.dsem = key
        self._deps(queue, [in_buf], [out_buf])
        e = self.eng[queue]
        if fn is None:
            o = out_ap if out_ap is not None else out_buf.ap
            i = in_ap if in_ap is not None else in_buf.ap
            e.dma_start(out=o, in_=i).then_inc(self.sems[sb.dsem], 16)
            sb.dcount += 16
        else:
            for ins in fn(e):
                ins.then_inc(self.sems[sb.dsem], 16)
                sb.dcount += 16
        tok = (sb.dsem, sb.dcount)
        self._commit(tok, [in_buf], [out_buf])
        return tok

    def finish(self, engine="sp"):
        for tok in self.out_tokens:
            self._wait(engine, tok)


def _tiles(s, name, shape, dtype, n):
    out = []
    for i in range(n):
        t = s.stack.enter_context(s.nc.sbuf_tensor("%s%d" % (name, i), shape, dtype))
        out.append(Buf(t[:], "%s%d" % (name, i)))
    return out


def _psum(s, name, n, shape=(128, 512), dtype=F32):
    out = []
    for i in range(n):
        t = s.stack.enter_context(s.nc.psum_tensor("%s%d" % (name, i), list(shape), dtype))
        out.append(Buf(t[:], "%s%d" % (name, i)))
    return out


class Ring:
    def __init__(self, bufs):
        self.bufs = bufs
        self.i = 0

    def next(self):
        b = self.bufs[self.i % len(self.bufs)]
        self.i += 1
        return b


def _layer_norm_F(s, z, mean_t, rstd_t, wk, sqr, pst, ones, lnp, gi, bi, emit_out):
    nc = s.nc
    for db in range(KC):
        sq = sqr.next()
        s.op("act", [z[db]], [sq],
             lambda e, db=db, sq=sq: e.activation(out=sq.ap, in_=z[db].ap, func=AF.Square))
        def mm(e, db=db, sq=sq):
            e.matmul(pst[0].ap, lhsT=ones.ap, rhs=z[db].ap, start=(db == 0), stop=(db == KC - 1))
            return e.matmul(pst[1].ap, lhsT=ones.ap, rhs=sq.ap, start=(db == 0), stop=(db == KC - 1))
        s.op("pe", [z[db], sq, ones], [pst[0], pst[1]], mm)
    msq = wk.next()
    var = wk.next()
    s.op("dve", [pst[0]], [mean_t],
         lambda e: e.tensor_scalar(out=mean_t.ap, in0=pst[0].ap, scalar1=1.0 / D_MODEL, scalar2=None, op0=ALU.mult))
    s.op("dve", [mean_t], [msq],
         lambda e: e.tensor_tensor(out=msq.ap, in0=mean_t.ap, in1=mean_t.ap, op=ALU.mult))
    s.op("dve", [pst[1], msq], [var],
         lambda e: e.scalar_tensor_tensor(out=var.ap, in0=pst[1].ap, scalar=1.0 / D_MODEL, in1=msq.ap,
                                          op0=ALU.mult, op1=ALU.subtract))
    s.op("dve", [var], [msq],
         lambda e: e.tensor_scalar(out=msq.ap, in0=var.ap, scalar1=LN_EPS, scalar2=None, op0=ALU.add))
    s.op("act", [msq], [var],
         lambda e: e.activation(out=var.ap, in_=msq.ap, func=AF.Sqrt))
    s.op("dve", [var], [rstd_t],
         lambda e: e.reciprocal(out=rstd_t.ap, in_=var.ap))
    for db in range(KC):
        u = wk.next()
        v = wk.next()
        s.op("dve", [z[db], mean_t], [u],
             lambda e, db=db, u=u: e.tensor_tensor(out=u.ap, in0=z[db].ap, in1=mean_t.ap, op=ALU.subtract))
        s.op("dve", [u, rstd_t, lnp], [v],
             lambda e, db=db, u=u, v=v: e.scalar_tensor_tensor(
                 out=v.ap, in0=u.ap, scalar=lnp.ap[:, gi, db:db + 1], in1=rstd_t.ap,
                 op0=ALU.mult, op1=ALU.mult))
        emit_out(db, v)


def build_F(kind):
    moe = (kind == "moe")
    nc = bass.Bass("TRN2", target_bir_lowering=False)
    stack = contextlib.ExitStack()
    with stack:
        s = Sched(nc, stack)
        dr = lambda name, shape, k="ExternalInput": nc.dram_tensor(name, shape, F32, kind=k).ap()
        mix = dr("mix", [4, 2, 8, 128, TT])
        hres = dr("hres", [2, KC, 128, TT])
        wout = dr("wout", [KC, 128, KC, 128])
        lnp_d = dr("lnp", [128, 4, KC])
        cst_d = dr("cst", [128, 2, 128])
        sel_d = dr("sel", [8, 8, 128])
        if moe:
            wg = dr("wg", [N_EXP, 16, 128, KC, 128])
            wu = dr("wu", [N_EXP, 16, 128, KC, 128])
            wd = dr("wd", [N_EXP, 16, 128, 16, 256])
            rout_d = dr("router", [128, KC, 8])
        else:
            wg = dr("wg", [64, 128, KC, 128])
            wu = dr("wu", [64, 128, KC, 128])
            wd = dr("wd", [2, KC, 128, KC, 128])
        hout = dr("hout", [2, KC, 128, TT], "ExternalOutput")
        D = lambda ap, name="": Buf(ap, name)
        mix_b = D(mix); hres_b = D(hres); wout_b = D(wout); lnp_db = D(lnp_d); cst_b = D(cst_d); sel_b = D(sel_d)
        wg_b = D(wg); wu_b = D(wu); wd_b = D(wd); hout_b = D(hout)

        act_t = stack.enter_context(nc.sbuf_tensor("act", [128, KC, TT], BF16))
        act = [Buf(act_t[:, k, :], "act%d" % k) for k in range(KC)]
        z_t = stack.enter_context(nc.sbuf_tensor("z", [128, KC, TT], F32))
        z = [Buf(z_t[:, k, :], "z%d" % k) for k in range(KC)]
        NH = 16 if moe else 32
        hid_t = stack.enter_context(nc.sbuf_tensor("hid", [128, NH, TT], BF16))
        hid = [Buf(hid_t[:, k, :], "hid%d" % k) for k in range(NH)]
        wring = Ring(_tiles(s, "w", [128, KC, 128], BF16, 5))
        hring = Ring(_tiles(s, "hr", [128, TT], F32, 2))
        oring = Ring(_tiles(s, "ost", [128, TT], F32, 2))
        wk = Ring(_tiles(s, "wk", [128, TT], F32, 6))
        sqr = Ring(_tiles(s, "sq", [128, TT], F32, 2))
        mean_t, rstd_t = _tiles(s, "stat", [128, TT], F32, 2)
        lnp = _tiles(s, "lnp", [128, 4, KC], F32, 1)[0]
        lnab = _tiles(s, "lnab", [128, KC], F32, 1)[0]
        ones = _tiles(s, "ones", [128, 128], F32, 1)[0]
        if moe:
            ident = _tiles(s, "ident", [128, 128], F32, 1)[0]
            sel = _tiles(s, "sel", [8, 8, 128], F32, 1)[0]
            rout = _tiles(s, "rout", [128, KC, 8], F32, 1)[0]
            lg = _tiles(s, "lg", [128, 4, 8], F32, 1)[0]
            gts = _tiles(s, "gts", [128, 4, 8], F32, 1)[0]
            m8 = _tiles(s, "m8", [128, 4, 8], F32, 1)[0]
            sm = _tiles(s, "sm", [128, 4, 8], F32, 3)
            gT = _tiles(s, "gT", [8, TT], F32, 1)[0]
            gbc = Ring(_tiles(s, "gbc", [128, TT], F32, 2))
        pg = Ring(_psum(s, "pg", 2)); pu = Ring(_psum(s, "pu", 2))
        pacc = Ring(_psum(s, "pacc", 2)); pst = _psum(s, "pst", 2)

        s.dma("sp", lnp, lnp_db)
        s.op("dve", [], [ones], lambda e: e.memset(ones.ap, 1.0))
        s.op("dve", [lnp], [lnab],
             lambda e: e.tensor_scalar(out=lnab.ap, in0=lnp.ap[:, 1, :], scalar1=ALPHA, scalar2=None, op0=ALU.mult))
        if moe:
            s.dma("sp", ident, cst_b, in_ap=cst_d[:, 0, :])
            s.dma("sp", sel, sel_b)
            s.dma("sp", rout, D(rout_d))

        def load_w(src_ap, src_buf, shape=None):
            slot = wring.next()
            o = slot.ap if shape is None else shape(slot.ap)
            s.dma("pool", slot, src_buf, out_ap=o, in_ap=src_ap)
            return slot

        def proj(wslot, rhs_list, pout, nk=KC, wsel=None):
            def mm(e):
                ins = None
                for k in range(nk):
                    l = wslot.ap[:, k, :] if wsel is None else wsel(wslot.ap, k)
                    ins = e.matmul(pout.ap, lhsT=l, rhs=rhs_list[k].ap, start=(k == 0), stop=(k == nk - 1))
                return ins
            s.op("pe", [wslot] + list(rhs_list[:nk]), [pout], mm)

        for ttl in range(2):
            for g in range(4):
                def ld(e, g=g):
                    return [e.dma_start(out=act_t[:, g * 8:(g + 1) * 8, :],
                                        in_=mix[g, ttl].rearrange("c p t -> p c t"))]
                blks = act[g * 8:(g + 1) * 8]
                s._deps("pool", [mix_b], blks)
                sb = blks[0]
                if sb.dsem is None:
                    key = "d%d" % s.nsem; s.nsem += 1
                    s.sems[key] = stack.enter_context(nc.semaphore(key)); sb.dsem = key
                for ins in ld(s.eng["pool"]):
                    ins.then_inc(s.sems[sb.dsem], 16); sb.dcount += 16
                s._commit((sb.dsem, sb.dcount), [mix_b], blks)
            for db in range(KC):
                wslot = load_w(wout[db], wout_b)
                hr = hring.next()
                s.dma("sp", hr, hres_b, in_ap=hres[ttl, db])
                pa = pacc.next()
                proj(wslot, act, pa)
                s.op("dve", [hr, pa], [z[db]],
                     lambda e, db=db, hr=hr, pa=pa: e.scalar_tensor_tensor(
                         out=z[db].ap, in0=hr.ap, scalar=ALPHA, in1=pa.ap, op0=ALU.mult, op1=ALU.add))
            def out1(db, v):
                s.op("act", [v, lnp], [act[db]],
                     lambda e: e.activation(out=act[db].ap, in_=v.ap, func=AF.Identity,
                                            bias=lnp.ap[:, 1, db:db + 1], scale=1.0))
                s.op("act", [v, lnab], [z[db]],
                     lambda e: e.activation(out=z[db].ap, in_=v.ap, func=AF.Identity,
                                            bias=lnab.ap[:, db:db + 1], scale=ALPHA))
            _layer_norm_F(s, z, mean_t, rstd_t, wk, sqr, pst, ones, lnp, 0, 1, out1)
            if not moe:
                for half in range(2):
                    for fb in range(32):
                        Fi = half * 32 + fb
                        wgs = load_w(wg[Fi], wg_b)
                        wus = load_w(wu[Fi], wu_b)
                        g_ps = pg.next(); u_ps = pu.next()
                        proj(wgs, act, g_ps)
                        proj(wus, act, u_ps)
                        sg = wk.next()
                        s.op("act", [g_ps], [sg],
                             lambda e, sg=sg, g_ps=g_ps: e.activation(out=sg.ap, in_=g_ps.ap, func=AF.Silu))
                        s.op("dve", [sg, u_ps], [hid[fb]],
                             lambda e, sg=sg, u_ps=u_ps, fb=fb: e.tensor_tensor(
                                 out=hid[fb].ap, in0=sg.ap, in1=u_ps.ap, op=ALU.mult))
                    for db in range(KC):
                        wds = load_w(wd[half, db], wd_b)
                        pa = pacc.next()
                        proj(wds, hid, pa)
                        s.op("dve", [z[db], pa], [z[db]],
                             lambda e, db=db, pa=pa: e.tensor_tensor(out=z[db].ap, in0=z[db].ap, in1=pa.ap, op=ALU.add))
            else:
                for sub in range(4):
                    def mm(e, sub=sub):
                        ins = None
                        for k in range(KC):
                            ins = e.matmul(pst[0].ap[:, sub * 8:(sub + 1) * 8],
                                           lhsT=z[k].ap[:, sub * 128:(sub + 1) * 128], rhs=rout.ap[:, k, :],
                                           start=(k == 0), stop=(k == KC - 1))
                        return ins
                    s.op("pe", list(z) + [rout], [pst[0]], mm)
                s.op("dve", [pst[0]], [lg],
                     lambda e: e.tensor_scalar(out=lg.ap.rearrange("p a b -> p (a b)"), in0=pst[0].ap[:, 0:32],
                                               scalar1=1.0 / ALPHA, scalar2=None, op0=ALU.mult))
                for sub in range(4):
                    s.op("dve", [lg], [m8], lambda e, sub=sub: e.max(out=m8.ap[:, sub, :], in_=lg.ap[:, sub, :]))
                def negm(e):
                    return e.tensor_scalar(out=sm[2].ap[:, :, 0], in0=m8.ap[:, :, 0], scalar1=-1.0, scalar2=None, op0=ALU.mult)
                s.op("dve", [m8], [sm[2]], negm)
                for sub in range(4):
                    s.op("dve", [lg, m8], [sm[0]],
                         lambda e, sub=sub: e.tensor_scalar(out=sm[0].ap[:, sub, :], in0=lg.ap[:, sub, :],
                                                            scalar1=m8.ap[:, sub, 1:2], scalar2=None, op0=ALU.is_ge))
                    s.op("act", [lg, sm[2]], [sm[1]],
                         lambda e, sub=sub: e.activation(out=sm[1].ap[:, sub, :], in_=lg.ap[:, sub, :], func=AF.Exp,
                                                         bias=sm[2].ap[:, sub, 0:1], scale=1.0))
                    s.op("act", [m8, sm[2]], [sm[2]],
                         lambda e, sub=sub: e.activation(out=sm[2].ap[:, sub, 1:2], in_=m8.ap[:, sub, 1:2], func=AF.Exp,
                                                         bias=sm[2].ap[:, sub, 0:1], scale=1.0))
                s.op("dve", [sm[2]], [sm[2]],
                     lambda e: e.tensor_scalar(out=sm[2].ap[:, :, 2], in0=sm[2].ap[:, :, 1], scalar1=1.0, scalar2=None, op0=ALU.add))
                s.op("dve", [sm[2]], [sm[2]],
                     lambda e: e.reciprocal(out=sm[2].ap[:, :, 3], in_=sm[2].ap[:, :, 2]))
                for sub in range(4):
                    s.op("dve", [sm[0], sm[1], sm[2]], [gts],
                         lambda e, sub=sub: e.scalar_tensor_tensor(
                             out=gts.ap[:, sub, :], in0=sm[1].ap[:, sub, :], scalar=sm[2].ap[:, sub, 3:4],
                             in1=sm[0].ap[:, sub, :], op0=ALU.mult, op1=ALU.mult))
                for sub in range(4):
                    s.op("pe", [gts, ident], [pst[1]],
                         lambda e, sub=sub: e.transpose(out=pst[1].ap[0:8, sub * 128:(sub + 1) * 128],
                                                        in_=gts.ap[:, sub, :], identity=ident.ap))
                s.op("dve", [pst[1]], [gT], lambda e: e.tensor_copy(out=gT.ap, in_=pst[1].ap[0:8, :]))
                if DEBUG and ttl == 0:
                    dbg = nc.dram_tensor("dbg", [128, 4, 8], F32, kind="ExternalOutput").ap()
                    dbg2 = nc.dram_tensor("dbg2", [128, 4, 8], F32, kind="ExternalOutput").ap()
                    dbg3 = nc.dram_tensor("dbg3", [8, TT], F32, kind="ExternalOutput").ap()
                    s.out_tokens.append(s.dma("sp", Buf(dbg), gts, sem_buf=gts))
                    s.out_tokens.append(s.dma("sp", Buf(dbg2), lg, sem_buf=lg))
                    s.out_tokens.append(s.dma("sp", Buf(dbg3), gT, sem_buf=gT))
                for ex in range(N_EXP):
                    gb = gbc.next()
                    ps = pst[ex % 2]
                    s.op("pe", [sel, gT], [ps],
                         lambda e, ex=ex, ps=ps: e.matmul(ps.ap, lhsT=sel.ap[:, ex, :], rhs=gT.ap, start=True, stop=True))
                    s.op("act", [ps], [gb], lambda e, ps=ps, gb=gb: e.activation(out=gb.ap, in_=ps.ap, func=AF.Identity))
                    for fb in range(16):
                        wgs = load_w(wg[ex, fb], wg_b)
                        wus = load_w(wu[ex, fb], wu_b)
                        g_ps = pg.next(); u_ps = pu.next()
                        proj(wgs, act, g_ps)
                        proj(wus, act, u_ps)
                        sg = wk.next(); t2 = wk.next()
                        s.op("act", [g_ps], [sg],
                             lambda e, sg=sg, g_ps=g_ps: e.activation(out=sg.ap, in_=g_ps.ap, func=AF.Silu))
                        s.op("dve", [sg, u_ps], [t2],
                             lambda e, sg=sg, u_ps=u_ps, t2=t2: e.tensor_tensor(out=t2.ap, in0=sg.ap, in1=u_ps.ap, op=ALU.mult))
                        s.op("dve", [t2, gb], [hid[fb]],
                             lambda e, t2=t2, gb=gb, fb=fb: e.tensor_tensor(out=hid[fb].ap, in0=t2.ap, in1=gb.ap, op=ALU.mult))
                    for dbp in range(16):
                        wds = load_w(wd[ex, dbp], wd_b,
                                     shape=lambda a: a.rearrange("p k c -> p (k c)").rearrange("p (f c) -> p f c", c=256))
                        for hb in range(2):
                            db = dbp * 2 + hb
                            pa = pacc.next()
                            proj(wds, hid, pa, nk=16,
                                 wsel=lambda a, k, hb=hb: a.rearrange("p k c -> p (k c)").rearrange(
                                     "p (f c) -> p f c", c=256)[:, k, hb * 128:(hb + 1) * 128])
                            s.op("dve", [z[db], pa], [z[db]],
                                 lambda e, db=db, pa=pa: e.tensor_tensor(out=z[db].ap, in0=z[db].ap, in1=pa.ap, op=ALU.add))
            def out2(db, v):
                o = oring.next()
                s.op("act", [v, lnp], [o],
                     lambda e: e.activation(out=o.ap, in_=v.ap, func=AF.Identity,
                                            bias=lnp.ap[:, 3, db:db + 1], scale=1.0))
                tok = s.dma("sp", hout_b, o, out_ap=hout[ttl, db], sem_buf=o)
                s.out_tokens.append(tok)
            _layer_norm_F(s, z, mean_t, rstd_t, wk, sqr, pst, ones, lnp, 2, 3, out2)
        s.finish("sp")
    return nc


def tile_w(w):
    K, N = w.shape
    return np.ascontiguousarray(w.reshape(K // 128, 128, N // 128, 128).transpose(2, 1, 0, 3))


def to_AT(h2d):
    n = h2d.shape[0]
    return np.ascontiguousarray(h2d.reshape(n // TT, TT, KC, 128).transpose(0, 2, 3, 1))


def from_AT(a):
    nt = a.shape[0]
    return np.ascontiguousarray(a.transpose(0, 3, 1, 2).reshape(nt * TT, D_MODEL))


def wout_rows():
    idx = np.empty(D_MODEL, np.int64)
    for g in range(4):
        for m in range(4):
            for blk in range(2):
                kc = g * 8 + m * 2 + blk
                idx[kc * 128:(kc + 1) * 128] = m * 1024 + g * 256 + blk * 128 + np.arange(128)
    return idx


def consts_F():
    cst = np.zeros((128, 2, 128), np.float32)
    cst[:, 0, :] = np.eye(128, dtype=np.float32)
    sel = np.zeros((8, 8, 128), np.float32)
    for e in range(8):
        sel[e, e, :] = 1.0
    return cst, sel


def prep_F(layer, inp):
    j = layer // 2
    d = {}
    d["wout"] = tile_w(inp["w_out"][layer][wout_rows(), :])
    d["lnp"] = np.ascontiguousarray(
        np.stack([inp["ln1_g"][layer], inp["ln1_b"][layer], inp["ln2_g"][layer], inp["ln2_b"][layer]])
        .reshape(4, KC, 128).transpose(2, 0, 1))
    d["cst"], d["sel"] = consts_F()
    if layer % 2 == 0:
        d["wg"] = tile_w(inp["ffn_w_gate"][j])
        d["wu"] = tile_w(inp["ffn_w_up"][j])
        d["wd"] = np.ascontiguousarray(
            inp["ffn_w_down"][j].reshape(2, 32, 128, 32, 128).transpose(0, 3, 2, 1, 4))
    else:
        d["wg"] = np.stack([tile_w(inp["exp_w_gate"][j][e]) for e in range(N_EXP)])
        d["wu"] = np.stack([tile_w(inp["exp_w_up"][j][e]) for e in range(N_EXP)])
        d["wd"] = np.stack([np.ascontiguousarray(
            inp["exp_w_down"][j][e].reshape(16, 128, 16, 256).transpose(2, 1, 0, 3)) for e in range(N_EXP)])
        d["router"] = np.ascontiguousarray(inp["router_w"][j].reshape(KC, 128, 8).transpose(1, 0, 2))
    return d


class View:
    __slots__ = ("buf", "ap")

    def __init__(self, buf, ap):
        self.buf = buf
        self.ap = ap


def _b(x):
    return x.buf if isinstance(x, View) else x


def _isv(x):
    return isinstance(x, (View, Buf))


def _sc(x):
    return x.ap if _isv(x) else x


class Ops:
    def __init__(self, s):
        self.s = s

    def act(self, out, in_, func, bias=None, scale=None):
        reads = [_b(in_)] + [_b(x) for x in (bias, scale) if _isv(x)]
        kw = {}
        if bias is not None:
            kw["bias"] = _sc(bias)
        if scale is not None:
            kw["scale"] = _sc(scale)
        return self.s.op("act", reads, [_b(out)],
                         lambda e: e.activation(out=out.ap, in_=in_.ap, func=func, **kw))

    def tt(self, out, a, b, op, eng="dve"):
        return self.s.op(eng, [_b(a), _b(b)], [_b(out)],
                         lambda e: e.tensor_tensor(out=out.ap, in0=a.ap, in1=b.ap, op=op))

    def ts(self, out, a, s1, op0, s2=None, op1=None, eng="dve"):
        reads = [_b(a)] + [_b(x) for x in (s1, s2) if _isv(x)]
        if op1 is None:
            f = lambda e: e.tensor_scalar(out=out.ap, in0=a.ap, scalar1=_sc(s1), scalar2=None, op0=op0)
        else:
            f = lambda e: e.tensor_scalar(out=out.ap, in0=a.ap, scalar1=_sc(s1), scalar2=_sc(s2), op0=op0, op1=op1)
        return self.s.op(eng, reads, [_b(out)], f)

    def stt(self, out, a, sc, b, op0, op1):
        reads = [_b(a), _b(b)] + ([_b(sc)] if _isv(sc) else [])
        return self.s.op("dve", reads, [_b(out)],
                         lambda e: e.scalar_tensor_tensor(out=out.ap, in0=a.ap, scalar=_sc(sc), in1=b.ap, op0=op0, op1=op1))

    def copy(self, out, in_, eng="dve"):
        return self.s.op(eng, [_b(in_)], [_b(out)], lambda e: e.tensor_copy(out=out.ap, in_=in_.ap))

    def recip(self, out, in_):
        return self.s.op("dve", [_b(in_)], [_b(out)], lambda e: e.reciprocal(out=out.ap, in_=in_.ap))

    def scan(self, out, d0, d1, init):
        reads = [_b(d0), _b(d1)] + ([_b(init)] if _isv(init) else [])
        return self.s.op("dve", reads, [_b(out)],
                         lambda e: e.tensor_tensor_scan(out=out.ap, data0=d0.ap, data1=d1.ap, initial=_sc(init),
                                                        op0=ALU.mult, op1=ALU.add))

    def mm(self, out, lhsT, rhs, start=True, stop=True):
        return self.s.op("pe", [_b(lhsT), _b(rhs)], [_b(out)],
                         lambda e: e.matmul(out.ap, lhsT=lhsT.ap, rhs=rhs.ap, start=start, stop=stop))

    def mmg(self, outs, lst):
        reads = []
        for (_, l, r, _, _) in lst:
            reads += [_b(l), _b(r)]
        def f(e):
            ins = None
            for (o, l, r, st, sp) in lst:
                ins = e.matmul(o.ap, lhsT=l.ap, rhs=r.ap, start=st, stop=sp)
            return ins
        return self.s.op("pe", reads, [_b(o) for o in outs], f)


NPP = 40
QSCALE = 128.0 ** -0.5


def build_M():
    nc = bass.Bass("TRN2", target_bir_lowering=False)
    stack = contextlib.ExitStack()
    with stack:
        s = Sched(nc, stack)
        o = Ops(s)
        dr = lambda name, shape, k="ExternalInput": nc.dram_tensor(name, shape, F32, kind=k).ap()
        hin = dr("hin", [8, KC, 128, TT]); hin_b = Buf(hin)
        win = dr("win", [13, 128, KC, 256]); win_b = Buf(win)
        wcr_d = dr("wcr", [128, KC, 16])
        pp_d = dr("pp", [128, NPP])
        pm_d = dr("pm", [128, 4, 128])
        gwg_d = dr("gwg", [16, 128])
        cm_d = dr("cm", [128, 7, 128])
        cd_d = dr("cd", [128, 5, TT])
        rope_d = dr("rope", [4, 128, SEQ]); rope_b = Buf(rope_d)
        mixo = dr("mixo", [4, 2, 8, 128, TT], "ExternalOutput"); mixo_b = Buf(mixo)

        T1 = lambda name, shape, dt=F32: _tiles(s, name, shape, dt, 1)[0]
        ht = T1("ht", [128, KC, TT], BF16)
        wring = Ring(_tiles(s, "w", [128, KC, 256], BF16, 3))
        wcr = T1("wcr", [128, KC, 16], BF16)
        pp = T1("pp", [128, NPP]); pm = T1("pm", [128, 4, 128]); gwg = T1("gwg", [16, 128])
        cm = T1("cm", [128, 7, 128]); cd = T1("cd", [128, 5, TT])
        der = T1("der", [128, 24])
        rp = [T1("rp%d" % i, [128, TT]) for i in range(4)]
        ecum = [T1("ecum%d" % i, [128, TT]) for i in range(2)]
        qt = [T1("qt%d" % i, [128, TT]) for i in range(2)]
        kt = [T1("kt%d" % i, [128, TT]) for i in range(2)]
        kht = [T1("kht%d" % i, [128, TT]) for i in range(2)]
        gate = [T1("gate%d" % i, [128, TT]) for i in range(2)]
        vT = [T1("vT%d" % i, [128, 256]) for i in range(4)]
        khT = [T1("khT%d" % i, [128, 128]) for i in range(4)]
        Pm = [T1("P%d" % i, [128, 128]) for i in range(4)]
        xpad = [T1("xpad%d" % i, [128, TT + 4]) for i in range(2)]
        hprev = T1("hprev", [128, 2])
        crT = T1("crT", [16, TT])
        wk = Ring(_tiles(s, "wk", [128, TT], F32, 10))
        ost = Ring(_tiles(s, "ost", [128, TT], F32, 2))
        SA = [Ring(_tiles(s, "SA%d_" % h, [128, 128], F32, 3)) for h in range(2)]
        SC = Ring(_tiles(s, "SC", [128, 256], F32, 3))
        SD = [Ring(_tiles(s, "SD%d_" % h, [128, 128], F32, 3)) for h in range(2)]
        pF = Ring(_psum(s, "pF", 3)); pM = Ring(_psum(s, "pM", 3)); pO = _psum(s, "pO", 2)

        V = View
        col = lambda t, a, b: V(t, t.ap[:, a:b])
        ppc = lambda i: V(pp, pp.ap[:, i:i + 1])
        derc = lambda i: V(der, der.ap[:, i:i + 1])
        cmm = lambda i: V(cm, cm.ap[:, i, :])
        cdd = lambda i: V(cd, cd.ap[:, i, :])
        cmask, maskD, pswap, ones128, ones256, ident = cmm(0), [cmm(1), cmm(2)], cmm(3), cmm(4), cmm(5), cmm(6)
        mask01, qdec, kdec = cdd(0), [cdd(1), cdd(2)], [cdd(3), cdd(4)]

        for t, d in ((pp, pp_d), (pm, pm_d), (gwg, gwg_d), (cm, cm_d), (cd, cd_d)):
            s.dma("sp", t, Buf(d))
        s.dma("pool", wcr, Buf(wcr_d))
        for r in SA + [SC] + SD:
            for b in r.bufs:
                s.op("dve", [], [b], lambda e, b=b: e.memset(b.ap, 0.0))
        for b in xpad + [hprev]:
            s.op("dve", [], [b], lambda e, b=b: e.memset(b.ap, 0.0))
        e8 = V(der, der.ap[:, 8:16]); n8 = V(der, der.ap[:, 16:24])
        o.act(e8, V(pp, pp.ap[:, 0:8]), AF.Exp)
        o.tt(n8, e8, V(pp, pp.ap[:, 8:16]), ALU.mult)
        AX = mybir.AxisListType.X
        s.op("dve", [der], [der], lambda e: e.reduce_sum(out=der.ap[:, 4:6], in_=der.ap[:, 16:24].rearrange("p (a b) -> p a b", b=4), axis=AX))
        s.op("dve", [der], [der], lambda e: e.reduce_sum(out=der.ap[:, 6:8], in_=der.ap[:, 8:16].rearrange("p (a b) -> p a b", b=4), axis=AX))
        o.recip(V(der, der.ap[:, 6:8]), V(der, der.ap[:, 6:8]))
        o.tt(V(der, der.ap[:, 0:2]), V(der, der.ap[:, 4:6]), V(der, der.ap[:, 6:8]), ALU.mult)
        o.ts(V(der, der.ap[:, 2:4]), V(der, der.ap[:, 0:2]), -1.0, ALU.mult, 1.0, ALU.add)
        o.act(V(der, der.ap[:, 16:18]), V(pp, pp.ap[:, 31:33]), AF.Exp, scale=-1.0)
        o.ts(V(der, der.ap[:, 16:18]), V(der, der.ap[:, 16:18]), 1.0, ALU.add)
        o.act(V(der, der.ap[:, 18:20]), V(der, der.ap[:, 16:18]), AF.Ln)
        o.ts(V(der, der.ap[:, 8:10]), V(der, der.ap[:, 18:20]), -8.0, ALU.mult)
        o.ts(V(der, der.ap[:, 10:12]), V(der, der.ap[:, 18:20]), -16.0, ALU.mult)
        o.ts(V(der, der.ap[:, 12:13]), V(pp, pp.ap[:, 33:34]), -1.0, ALU.mult)
        lb = [derc(0), derc(1)]; oml = [derc(2), derc(3)]
        c8 = [derc(8), derc(9)]; c16 = [derc(10), derc(11)]; nbg = derc(12)

        def projF(W, blk, n=128, wt=None):
            p = pF.next()
            def f(e):
                ins = None
                for k in range(KC):
                    l = W.ap[:, k, blk * 128:blk * 128 + n] if wt is None else wt.ap[:, k, :]
                    ins = e.matmul(p.ap[0:n, :], lhsT=l, rhs=ht.ap[:, k, :], start=(k == 0), stop=(k == KC - 1))
                return ins
            s.op("pe", [W if wt is None else wt, ht], [p], f)
            return p

        def projT(W, sub):
            p = pM.next()
            def f(e):
                ins = None
                for k in range(KC):
                    ins = e.matmul(p.ap[:, 0:256], lhsT=ht.ap[:, k, sub * 128:(sub + 1) * 128], rhs=W.ap[:, k, :],
                                   start=(k == 0), stop=(k == KC - 1))
                return ins
            s.op("pe", [W, ht], [p], f)
            return p

        def load_v(W):
            for sub in range(4):
                pv = projT(W, sub)
                o.act(vT[sub], V(pv, pv.ap[:, 0:256]), AF.Identity)

        def khat_from(ktile, khtile, dl):
            for c in range(8):
                o.act(col(khtile, 64 * c, 64 * c + 64), col(ktile, 64 * c, 64 * c + 64), AF.Identity, scale=dl(c))

        def emit_out(ob, tt_i, cb):
            j, ttl = tt_i // 2, tt_i % 2
            tok = s.dma("sp", mixo_b, ob, out_ap=mixo[j, ttl, cb], sem_buf=ob)
            s.out_tokens.append(tok)

        def chain(qs, qi, ks, kh, vc0, nv, Sring, state, mask, dl):
            Vd = nv * 128
            for sub in range(4):
                c0, c1 = sub * 128, sub * 128 + 128
                pT = pM.next()
                s.op("pe", [kh, cm], [pT],
                     lambda e, pT=pT: e.transpose(out=pT.ap[:, 0:128], in_=kh.ap[:, c0:c1], identity=ident.ap))
                o.act(khT[sub], V(pT, pT.ap[:, 0:128]), AF.Identity)
                pS = pM.next()
                o.mm(V(pS, pS.ap[:, 0:128]), col(ks, c0, c1), col(qs, c0, c1))
                o.tt(Pm[sub], V(pS, pS.ap[:, 0:128]), mask, ALU.mult)
                Sb = []
                for half in range(2):
                    c = 2 * sub + half
                    Sprev = state["S"]
                    Sb.append(Sprev)
                    pU = pM.next()
                    o.mm(V(pU, pU.ap[:, 0:Vd]), V(khT[sub], khT[sub].ap[64 * half:64 * half + 64, :]),
                         V(vT[sub], vT[sub].ap[64 * half:64 * half + 64, vc0:vc0 + Vd]))
                    Sn = Sring.next()
                    o.stt(Sn, Sprev, dl(c), V(pU, pU.ap[:, 0:Vd]), ALU.mult, ALU.add)
                    state["S"] = Sn
                for vh in range(nv):
                    po = pO[vh]
                    o.mmg([po], [
                        (V(po, po.ap[:, c0:c1]), V(vT[sub], vT[sub].ap[:, vc0 + vh * 128:vc0 + vh * 128 + 128]), Pm[sub], True, False),
                        (V(po, po.ap[:, c0:c0 + 64]), V(Sb[0], Sb[0].ap[:, vh * 128:vh * 128 + 128]), col(qi, c0, c0 + 64), False, False),
                        (V(po, po.ap[:, c0 + 64:c1]), V(Sb[1], Sb[1].ap[:, vh * 128:vh * 128 + 128]), col(qi, c0 + 64, c1), False, True),
                    ])

        def rstd_from(pms, eps):
            a = wk.next(); b2 = wk.next(); c = wk.next()
            o.ts(a, pms, eps, ALU.add)
            o.act(b2, a, AF.Sqrt)
            o.recip(c, b2)
            return c

        stA = [{"S": SA[0].next()}, {"S": SA[1].next()}]
        stC = {"S": SC.next()}
        stD = [{"S": SD[0].next()}, {"S": SD[1].next()}]

        def A_f(W, ti):
            for hl in range(2):
                pf = projF(W, hl)
                sig = wk.next(); f = wk.next(); lgf = wk.next(); cum = wk.next(); enc = wk.next(); kk = wk.next()
                o.act(sig, pf, AF.Sigmoid)
                o.ts(f, sig, oml[hl], ALU.mult, lb[hl], ALU.add)
                o.act(lgf, f, AF.Ln)
                o.scan(cum, mask01, lgf, 0.0)
                o.act(ecum[hl], cum, AF.Exp)
                o.act(enc, cum, AF.Exp, scale=-1.0)
                o.ts(kk, f, -1.0, ALU.mult, 1.0, ALU.add)
                o.tt(kt[hl], kk, enc, ALU.mult)
                khat_from(kt[hl], kht[hl], lambda c, hl=hl: col(ecum[hl], 64 * c + 63, 64 * c + 64))

        def A_q(W, ti):
            for hl in range(2):
                pq = projF(W, hl)
                sq = wk.next()
                o.act(sq, pq, AF.Silu)
                o.stt(qt[hl], sq, QSCALE, ecum[hl], ALU.mult, ALU.mult)

        def A_v(W, ti):
            load_v(W)

        def A_g(W, ti):
            for hl in range(2):
                pg_ = projF(W, hl)
                o.act(gate[hl], pg_, AF.Silu)
            for hl in range(2):
                chain(qt[hl], qt[hl], kt[hl], kht[hl], hl * 128, 1, SA[hl], stA[hl], cmask,
                      lambda c, hl=hl: col(ecum[hl], 64 * c + 63, 64 * c + 64))
                po = pO[0]
                o2 = wk.next()
                o.act(o2, po, AF.Square)
                pms = pM.next()
                o.mm(pms, ones128, o2)
                rs = rstd_from(pms, RMS_EPS)
                on = wk.next()
                o.stt(on, po, ppc(16), rs, ALU.mult, ALU.mult)
                ob = ost.next()
                o.tt(ob, on, gate[hl], ALU.mult)
                emit_out(ob, ti, hl)

        def B_x(W, ti):
            for blk in range(2):
                px = projF(W, blk)
                xp = xpad[blk]
                o.act(col(xp, 3, 3 + TT), px, AF.Identity)
                xc = qt[blk]
                o.ts(xc, col(xp, 0, TT), ppc(17 + blk * 4), ALU.mult, ppc(25 + blk), ALU.add)
                for j in range(1, 4):
                    o.stt(xc, col(xp, j, j + TT), ppc(17 + blk * 4 + j), xc, ALU.mult, ALU.add)
                o.copy(col(xp, 0, 3), col(xp, TT, TT + 3))
                pr = pM.next()
                o.mm(pr, V(pm, pm.ap[:, blk, :]), xc)
                r = wk.next()
                o.act(r, pr, AF.Sigmoid, bias=ppc(27 + blk))
                pi = pM.next()
                o.mm(pi, V(pm, pm.ap[:, 2 + blk, :]), xc)
                ig = wk.next()
                o.act(ig, pi, AF.Sigmoid, bias=ppc(29 + blk))
                a = kt[blk]
                o.act(a, r, AF.Exp, scale=c8[blk])
                a2 = wk.next(); om = wk.next(); mlt = wk.next(); u = wk.next()
                o.act(a2, r, AF.Exp, scale=c16[blk])
                o.ts(om, a2, -1.0, ALU.mult, 1.0, ALU.add)
                o.act(mlt, om, AF.Sqrt)
                o.tt(u, mlt, ig, ALU.mult)
                o.tt(u, u, xc, ALU.mult)
                hh = kht[blk]
                o.scan(hh, a, u, V(hprev, hprev.ap[:, blk:blk + 1]))
                o.copy(V(hprev, hprev.ap[:, blk:blk + 1]), col(hh, TT - 1, TT))

        def B_y(W, ti):
            for blk in range(2):
                py = projF(W, blk)
                yy = wk.next(); y2 = wk.next(); w = wk.next(); sg = wk.next(); gl = wk.next()
                o.act(yy, py, AF.Identity)
                o.act(y2, py, AF.Square)
                o.ts(w, y2, 0.044715, ALU.mult, 1.0, ALU.add)
                o.tt(w, w, yy, ALU.mult)
                o.act(sg, w, AF.Sigmoid, scale=1.5957691216057308)
                o.tt(gl, yy, sg, ALU.mult)
                ob = ost.next()
                o.tt(ob, kht[blk], gl, ALU.mult)
                emit_out(ob, ti, 2 + blk)

        def C_qk(W, ti):
            pcr = projF(None, 0, n=16, wt=wcr)
            o.act(crT, V(pcr, pcr.ap[0:16, :]), AF.Identity)
            pgl = pM.next()
            o.mm(pgl, gwg, crT)
            en = wk.next(); sp = wk.next(); cum = wk.next(); enc = wk.next()
            o.act(en, pgl, AF.Exp, bias=nbg, scale=-1.0)
            o.ts(en, en, 1.0, ALU.add)
            o.act(sp, en, AF.Ln)
            o.scan(cum, mask01, sp, 0.0)
            o.act(ecum[0], cum, AF.Exp, scale=-1.0 / 16.0)
            o.act(enc, cum, AF.Exp, scale=1.0 / 16.0)
            pq = projF(W, 0)
            o.stt(qt[0], pq, QSCALE, ecum[0], ALU.mult, ALU.mult)
            pk = projF(W, 1)
            o.tt(kt[0], pk, enc, ALU.mult)
            khat_from(kt[0], kht[0], lambda c: col(ecum[0], 64 * c + 63, 64 * c + 64))

        def C_v(W, ti):
            load_v(W)

        def C_g(W, ti):
            for vh in range(2):
                pg_ = projF(W, vh)
                o.act(gate[vh], pg_, AF.Silu)
            chain(qt[0], qt[0], kt[0], kht[0], 0, 2, SC, stC, cmask,
                  lambda c: col(ecum[0], 64 * c + 63, 64 * c + 64))
            o2a = wk.next(); o2b = wk.next()
            o.act(o2a, pO[0], AF.Square)
            o.act(o2b, pO[1], AF.Square)
            pms = pM.next()
            o.mmg([pms], [(pms, ones256, o2a, True, False), (pms, ones256, o2b, False, True)])
            rs = rstd_from(pms, RMS_EPS)
            for vh in range(2):
                on = wk.next()
                o.stt(on, pO[vh], ppc(34 + vh), rs, ALU.mult, ALU.mult)
                ob = ost.next()
                o.tt(ob, on, gate[vh], ALU.mult)
                emit_out(ob, ti, 4 + vh)

        def rot(W, dst, ci, si):
            for hl in range(2):
                px = projF(W, hl)
                x = wk.next(); t1 = wk.next(); t2 = wk.next()
                o.act(x, px, AF.Identity)
                pxs = pM.next()
                o.mm(pxs, pswap, x)
                o.tt(t1, x, rp[ci], ALU.mult)
                o.tt(t2, pxs, rp[si], ALU.mult)
                o.tt(dst[hl], t1, t2, ALU.add)

        def D_q(W, ti):
            rot(W, ecum, 0, 1)
            for hl in range(2):
                o.tt(qt[hl], ecum[hl], qdec[hl], ALU.mult)

        def D_k(W, ti):
            rot(W, kt, 2, 3)
            for hl in range(2):
                o.tt(kht[hl], kt[hl], kdec[hl], ALU.mult)

        def D_v(W, ti):
            load_v(W)

        def D_g(W, ti):
            for hl in range(2):
                pg_ = projF(W, hl)
                o.act(gate[hl], pg_, AF.Silu)
            for hl in range(2):
                chain(ecum[hl], qt[hl], kt[hl], kht[hl], hl * 128, 1, SD[hl], stD[hl], maskD[hl],
                      lambda c, hl=hl: ppc(37 + hl))
                po = pO[0]
                osb = wk.next(); o2 = wk.next(); mu = wk.next(); msq = wk.next(); var = wk.next()
                o.act(osb, po, AF.Identity)
                o.act(o2, po, AF.Square)
                pmu = pM.next(); pms = pM.next()
                o.mm(pmu, ones128, osb)
                o.mm(pms, ones128, o2)
                o.act(mu, pmu, AF.Identity)
                o.tt(msq, mu, mu, ALU.mult)
                o.tt(var, pms, msq, ALU.subtract)
                rs = rstd_from(var, LN_EPS)
                cen = wk.next()
                o.tt(cen, osb, mu, ALU.subtract)
                on = wk.next()
                o.stt(on, cen, ppc(36), rs, ALU.mult, ALU.mult)
                ob = ost.next()
                o.tt(ob, on, gate[hl], ALU.mult)
                emit_out(ob, ti, 6 + hl)

        order = [(1, A_f), (0, A_q), (3, A_v), (2, A_g), (4, B_x), (5, B_y), (6, C_qk), (8, C_v), (7, C_g),
                 (9, D_q), (10, D_k), (12, D_v), (11, D_g)]
        jobs = [(ti, G, fn) for ti in range(8) for (G, fn) in order]
        slots = {}

        def issue_load(i):
            if i < len(jobs):
                _, G, _ = jobs[i]
                slot = wring.next()
                s.dma("pool", slot, win_b, in_ap=win[G])
                slots[i] = slot

        def load_tile(ti):
            for q4 in range(4):
                s.dma("pool", ht, hin_b, out_ap=ht.ap[:, q4 * 8:(q4 + 1) * 8, :],
                      in_ap=hin[ti, q4 * 8:(q4 + 1) * 8].rearrange("c p t -> p c t"))
            for i in range(4):
                s.dma("sp", rp[i], rope_b, in_ap=rope_d[i, :, ti * TT:(ti + 1) * TT])

        issue_load(0); issue_load(1)
        for i, (ti, G, fn) in enumerate(jobs):
            if G == 1:
                load_tile(ti)
            issue_load(i + 2)
            fn(slots.pop(i), ti)
        s.finish("sp")
    return nc


def m_groups(g):
    r = lambda a, n=256: np.arange(a, a + n)
    A0, B0, C0, D0 = 0, 4096, 6144, 9232
    return [
        r(A0 + 0 + 256 * g), r(A0 + 1024 + 256 * g), r(A0 + 3072 + 256 * g), r(A0 + 2048 + 256 * g),
        r(B0 + 256 * g), r(B0 + 1024 + 256 * g),
        np.concatenate([r(C0 + 128 * g, 128), r(C0 + 512 + 128 * g, 128)]), r(C0 + 2048 + 256 * g), r(C0 + 1024 + 256 * g),
        r(D0 + 256 * g), r(D0 + 1024 + 256 * g), r(D0 + 3072 + 256 * g), r(D0 + 2048 + 256 * g),
    ]


_CONST_CACHE = {}


def consts_M(g):
    if g in _CONST_CACHE:
        return _CONST_CACHE[g]
    f32 = np.float32
    cm = np.zeros((128, 7, 128), f32)
    sidx = np.arange(128)[:, None]; tidx = np.arange(128)[None, :]
    causal = ((sidx // 64) == (tidx // 64)) & (sidx <= tidx)
    cm[:, 0, :] = causal
    cd = np.zeros((128, 5, TT), f32)
    tl = np.arange(TT) % 64
    cd[:, 0, :] = (tl != 0).astype(f32)[None, :]
    gam64 = np.zeros(2, f32)
    for hl in range(2):
        h = 2 * g + hl
        lgam = np.log1p(-(2.0 ** (-5.0 - h)))
        cm[:, 1 + hl, :] = np.where(causal, np.exp(lgam * np.maximum(tidx - sidx, 0)), 0.0)
        cd[:, 1 + hl, :] = np.exp(lgam * (tl + 1.0))[None, :]
        cd[:, 3 + hl, :] = np.exp(lgam * (63.0 - tl))[None, :]
        gam64[hl] = np.exp(lgam * 64.0)
    m = np.arange(128)
    cm[(m + 64) % 128, 3, m] = 1.0
    cm[:, 4, :] = 1.0 / 128.0
    cm[:, 5, :] = 1.0 / 256.0
    cm[:, 6, :] = np.eye(128, dtype=f32)
    half = 64
    inv_freq = (f32(10000.0) ** (-(np.arange(half, dtype=f32)) / f32(half))).astype(f32)
    ang = (np.arange(SEQ, dtype=f32)[:, None] * inv_freq[None, :]).astype(f32)
    cos = np.cos(ang).astype(f32).T; sin = np.sin(ang).astype(f32).T
    rope = np.zeros((4, 128, SEQ), f32)
    rope[0] = np.concatenate([cos, cos]); rope[1] = np.concatenate([-sin, sin])
    ks = f32(128.0 ** -0.5)
    rope[2] = rope[0] * ks; rope[3] = rope[1] * ks
    _CONST_CACHE[g] = (cm, cd, rope, gam64)
    return _CONST_CACHE[g]


def prep_M(layer, inp, g):
    d = {}
    w = inp["w_in"][layer]
    d["win"] = np.stack([np.ascontiguousarray(w[:, c].reshape(KC, 128, 256).transpose(1, 0, 2)) for c in m_groups(g)])
    d["wcr"] = np.ascontiguousarray(w[:, 9216:9232].reshape(KC, 128, 16).transpose(1, 0, 2))
    cm, cd, rope, gam64 = consts_M(g)
    d["cm"], d["cd"], d["rope"] = cm, cd, rope
    pp = np.zeros((128, NPP), np.float32)
    for hl in range(2):
        h = 2 * g + hl
        pp[:, hl * 4:(hl + 1) * 4] = inp["hgrn_lb"][:, h * 128:(h + 1) * 128].T
        pp[:, 8 + hl * 4:8 + (hl + 1) * 4] = (np.arange(4) >= 1) & (np.arange(4) <= layer)
        pp[:, 37 + hl] = gam64[hl]
    pp[:, 16] = inp["hgrn_norm"][layer]
    for blk in range(2):
        ch = slice((2 * g + blk) * 128, (2 * g + blk + 1) * 128)
        pp[:, 17 + blk * 4:21 + blk * 4] = inp["conv_w"][layer][:, ch].T
        pp[:, 25 + blk] = inp["conv_b"][layer][ch]
        pp[:, 27 + blk] = inp["lru_ba"][layer][ch]
        pp[:, 29 + blk] = inp["lru_bx"][layer][ch]
        pp[:, 31 + blk] = inp["lru_lambda"][layer][ch]
        pp[:, 34 + blk] = inp["gla_norm"][layer][blk * 128:(blk + 1) * 128]
    pp[:, 33] = inp["gla_b_gate"][layer][g * 128:(g + 1) * 128]
    pp[:, 36] = inp["ret_norm"][layer]
    d["pp"] = pp
    d["pm"] = np.ascontiguousarray(np.stack([inp["lru_wa"][layer][2 * g], inp["lru_wa"][layer][2 * g + 1],
                                             inp["lru_wx"][layer][2 * g], inp["lru_wx"][layer][2 * g + 1]], axis=1))
    d["gwg"] = np.ascontiguousarray(inp["gla_w_gate"][layer][:, g * 128:(g + 1) * 128])
    return d


_PROGS = {}


def _prog(name):
    if name not in _PROGS:
        _PROGS[name] = build_M() if name == "M" else build_F(name)
    return _PROGS[name]


def kernel(**inputs):
    inp = {k: np.asarray(v) for k, v in inputs.items()}
    cores = list(range(NCORES))
    hAT = to_AT(np.ascontiguousarray(inp["x"], dtype=np.float32).reshape(BATCH * SEQ, D_MODEL))
    for layer in range(DEPTH):
        pm = [prep_M(layer, inp, g) for g in range(4)]
        in_maps = []
        for c in cores:
            b, g = c // 4, c % 4
            d = dict(pm[g])
            d["hin"] = np.ascontiguousarray(hAT[8 * b:8 * b + 8])
            in_maps.append(d)
        res = run_bass_kernel_spmd(_prog("M"), in_maps, core_ids=cores)
        mixo = [np.asarray(r["mixo"]) for r in res.results]
        del pm, in_maps
        pf = prep_F(layer, inp)
        in_maps = []
        for c in cores:
            b, j = c // 4, c % 4
            d = dict(pf)
            d["mix"] = np.ascontiguousarray(np.stack([mixo[4 * b + g][j] for g in range(4)]))
            d["hres"] = np.ascontiguousarray(hAT[2 * c:2 * c + 2])
            in_maps.append(d)
        res = run_bass_kernel_spmd(_prog("dense" if layer % 2 == 0 else "moe"), in_maps, core_ids=cores)
        hAT = np.concatenate([np.asarray(r["hout"]) for r in res.results], axis=0)
        del pf, in_maps
    out = from_AT(hAT).reshape(BATCH, SEQ, D_MODEL)
    return np.ascontiguousarray(out.astype(np.float32))
```
